# Optimizing a Trainium2 kernel written in Bass

```python
import math, functools
import jax, jax.numpy as jnp
from jax import lax
import numpy as np

D_MODEL = 1024
BATCH = 2
SEQ = 16384
DEPTH = 4

MEM_TOKENS = 256
HEAD_DIM = 64
ROPE_THETA = 500000.0
PARTIAL_ROT = HEAD_DIM // 4
Q_BLOCK = 128
RMS_EPS = 1e-6
LN_EPS = 1e-5
DSA_HEADS = 8
DSA_TOPK = 256
IDX_HEADS = 4
IDX_DIM = 64
MLA_HEADS = 8
MLA_Q_RANK = 384
MLA_KV_RANK = 256
MLA_NOPE = 64
MLA_ROPE = 32
MLA_V = 64
DIFF_HEADS = 4
DIFF_DIM = 64
SGU_CHUNK = 128
SGU_GROUPS = 8
SGU_WIDTH = 512
MEM_HEADS = 4
MEM_DIM = 128
N_BRANCH = 5
BRANCH_WIDTH = 512

SPLIT_SIZES = (
    DSA_HEADS * HEAD_DIM, HEAD_DIM, HEAD_DIM,
    IDX_HEADS * IDX_DIM, IDX_DIM, IDX_HEADS,
    MLA_Q_RANK, MLA_KV_RANK, MLA_ROPE,
    2 * DIFF_HEADS * DIFF_DIM, 2 * DIFF_HEADS * DIFF_DIM, DIFF_HEADS * 2 * DIFF_DIM,
    SGU_WIDTH, SGU_WIDTH,
    MEM_HEADS * MEM_DIM,
    N_BRANCH * BRANCH_WIDTH,
    N_BRANCH * D_MODEL,
)
IN_WIDTH = sum(SPLIT_SIZES)

kernel_name = 'hybrid_dsa_mla_diff_sgu_mem_block'


def rms_norm(x, g, eps=RMS_EPS):
    xf = x.astype(jnp.float32)
    y = xf * lax.rsqrt(jnp.mean(xf * xf, axis=-1, keepdims=True) + eps)
    return (y * g.astype(jnp.float32)).astype(x.dtype)


def layer_norm(x, g, b, eps=LN_EPS):
    xf = x.astype(jnp.float32)
    mu = jnp.mean(xf, axis=-1, keepdims=True)
    xc = xf - mu
    y = xc * lax.rsqrt(jnp.mean(xc * xc, axis=-1, keepdims=True) + eps)
    return (y * g.astype(jnp.float32) + b.astype(jnp.float32)).astype(x.dtype)


def rope_tables(positions, rot_dim):
    inv_freq = ROPE_THETA ** (-jnp.arange(0, rot_dim, 2, dtype=jnp.float32) / rot_dim)
    ang = positions.astype(jnp.float32)[..., None] * inv_freq
    return jnp.cos(ang), jnp.sin(ang)


def apply_rope(x, cos, sin):
    half = cos.shape[-1]
    c = cos[:, :, None, :].astype(x.dtype)
    s = sin[:, :, None, :].astype(x.dtype)
    x1 = x[..., :half]
    x2 = x[..., half:2 * half]
    return jnp.concatenate([x1 * c - x2 * s, x2 * c + x1 * s, x[..., 2 * half:]], axis=-1)


def _to_blocks(t, nb):
    return t.reshape((t.shape[0], nb, Q_BLOCK) + t.shape[2:]).swapaxes(0, 1)


def _from_blocks(t):
    nb, b, q = t.shape[:3]
    return t.swapaxes(0, 1).reshape((b, nb * q) + t.shape[3:])


def causal_attention(q, k, v, scale, mix=None):
    S = q.shape[1]
    nb = S // Q_BLOCK
    key_pos = jnp.arange(S)

    def body(args):
        qb, i = args
        q_pos = i * Q_BLOCK + jnp.arange(Q_BLOCK)
        s = jnp.einsum('bqhd,bkhd->bhqk', qb, k).astype(jnp.float32) * scale
        s = jnp.where(key_pos[None, :] <= q_pos[:, None], s, -jnp.inf)
        p = jax.nn.softmax(s, axis=-1)
        if mix is not None:
            p = mix(p)
        return jnp.einsum('bhqk,bkhd->bqhd', p.astype(v.dtype), v)

    return _from_blocks(lax.map(body, (_to_blocks(q, nb), jnp.arange(nb))))


def dsa_attention(q, k, v, qi, ki, wi):
    B, S, H, dh = q.shape
    k_sel = min(DSA_TOPK, S // 4)
    nb = S // Q_BLOCK
    key_pos = jnp.arange(S)
    gather = jax.vmap(lambda t, j: t[j])

    def body(args):
        qb, qib, wib, i = args
        q_pos = i * Q_BLOCK + jnp.arange(Q_BLOCK)
        dots = jnp.einsum('bqhd,bkd->bqhk', qib, ki).astype(jnp.float32)
        score = jnp.einsum('bqh,bqhk->bqk', wib.astype(jnp.float32), jax.nn.relu(dots))
        score = jnp.where(key_pos[None, None, :] <= q_pos[None, :, None], score, -jnp.inf)
        _, idx = lax.top_k(score, k_sel)
        valid = idx <= q_pos[None, :, None]
        k_g = gather(k, idx)
        v_g = gather(v, idx)
        s = jnp.einsum('bqhd,bqkd->bqhk', qb, k_g).astype(jnp.float32) * dh ** -0.5
        s = jnp.where(valid[:, :, None, :], s, -jnp.inf)
        p = jax.nn.softmax(s, axis=-1)
        return jnp.einsum('bqhk,bqkd->bqhd', p.astype(v.dtype), v_g)

    out = lax.map(body, (_to_blocks(q, nb), _to_blocks(qi, nb), _to_blocks(wi, nb), jnp.arange(nb)))
    return _from_blocks(out)


def dsa_branch(a_q, a_k, a_v, i_q, i_k, i_w, cos, sin):
    B, S, _ = a_q.shape
    q = apply_rope(a_q.reshape(B, S, DSA_HEADS, HEAD_DIM), cos, sin)
    k = apply_rope(a_k[:, :, None, :], cos, sin)[:, :, 0]
    qi = apply_rope(i_q.reshape(B, S, IDX_HEADS, IDX_DIM), cos, sin)
    ki = apply_rope(i_k[:, :, None, :], cos, sin)[:, :, 0]
    wi = i_w * (IDX_HEADS * IDX_DIM) ** -0.5
    return dsa_attention(q, k, a_v, qi, ki, wi).reshape(B, S, DSA_HEADS * HEAD_DIM)


def mla_branch(cq, ckv, kr, q_norm_g, kv_norm_g, w_uq, w_ukv, cos, sin):
    B, S, _ = cq.shape
    q = (rms_norm(cq, q_norm_g) @ w_uq).reshape(B, S, MLA_HEADS, MLA_NOPE + MLA_ROPE)
    q = jnp.concatenate([q[..., :MLA_NOPE], apply_rope(q[..., MLA_NOPE:], cos, sin)], axis=-1)
    kv = (rms_norm(ckv, kv_norm_g) @ w_ukv).reshape(B, S, MLA_HEADS, MLA_NOPE + MLA_V)
    k_rope = apply_rope(kr[:, :, None, :], cos, sin)
    k = jnp.concatenate([kv[..., :MLA_NOPE], jnp.broadcast_to(k_rope, (B, S, MLA_HEADS, MLA_ROPE))], axis=-1)
    o = causal_attention(q, k, kv[..., MLA_NOPE:], (MLA_NOPE + MLA_ROPE) ** -0.5)
    return o.reshape(B, S, MLA_HEADS * MLA_V)


def diff_branch(cq, ck, cv, lam_params, sub_norm_g, layer, cos, sin):
    B, S, _ = cq.shape
    lam_init = 0.8 - 0.6 * math.exp(-0.3 * layer)
    lp = lam_params.astype(jnp.float32)
    lam = jnp.exp(jnp.sum(lp[0] * lp[1])) - jnp.exp(jnp.sum(lp[2] * lp[3])) + lam_init
    q = apply_rope(cq.reshape(B, S, 2 * DIFF_HEADS, DIFF_DIM), cos, sin)
    k = apply_rope(ck.reshape(B, S, 2 * DIFF_HEADS, DIFF_DIM), cos, sin)
    v = cv.reshape(B, S, DIFF_HEADS, 2 * DIFF_DIM)

    def diff_mix(p):
        b, h2, nq, nk = p.shape
        pr = p.reshape(b, h2 // 2, 2, nq, nk)
        return pr[:, :, 0] - lam * pr[:, :, 1]

    o = causal_attention(q, k, v, DIFF_DIM ** -0.5, diff_mix)
    o = rms_norm(o, sub_norm_g) * (1.0 - lam_init)
    return o.reshape(B, S, DIFF_HEADS * 2 * DIFF_DIM)


def sgu_branch(u, v, ln_g, ln_b, w_s, b_s):
    B, S, _ = u.shape
    nc = S // SGU_CHUNK
    vc = layer_norm(v, ln_g, ln_b).reshape(B, nc, SGU_CHUNK, SGU_GROUPS, SGU_WIDTH // SGU_GROUPS)
    w_causal = w_s * jnp.tril(jnp.ones((SGU_CHUNK, SGU_CHUNK), w_s.dtype))
    z = jnp.einsum('gij,bcjgd->bcigd', w_causal, vc) + b_s.T[None, None, :, :, None]
    return u * z.reshape(B, S, SGU_WIDTH)


def memory_branch(eq, mem_n, w_kv):
    B, S, _ = eq.shape
    M = mem_n.shape[1]
    kv = (mem_n @ w_kv).reshape(B, M, 2, MEM_HEADS, MEM_DIM)
    q = eq.reshape(B, S, MEM_HEADS, MEM_DIM)
    s = jnp.einsum('bshd,bmhd->bhsm', q, kv[:, :, 0]).astype(jnp.float32) * MEM_DIM ** -0.5
    p = jax.nn.softmax(s, axis=-1)
    o = jnp.einsum('bhsm,bmhd->bshd', p.astype(kv.dtype), kv[:, :, 1])
    return o.reshape(B, S, MEM_HEADS * MEM_DIM)


def setup_inputs(seed: int = 0) -> dict:
    key = jax.random.key(seed)
    ks = jax.random.split(key, 24)
    f32 = jnp.float32

    def nrm(k, shape, scale):
        return jax.random.normal(k, shape, f32) * scale

    def gain(k, shape):
        return 1.0 + 0.02 * jax.random.normal(k, shape, f32)

    offs = jax.random.randint(ks[2], (BATCH, 1), 0, 4096)
    positions = (offs + jnp.arange(SEQ)[None, :]).astype(jnp.int32)
    return {
        'x': nrm(ks[0], (BATCH, SEQ, D_MODEL), 1.0),
        'mem': nrm(ks[1], (BATCH, MEM_TOKENS, D_MODEL), 1.0),
        'positions': positions,
        'norm_g': gain(ks[3], (DEPTH, D_MODEL)),
        'w_in': nrm(ks[4], (DEPTH, D_MODEL, IN_WIDTH), D_MODEL ** -0.5),
        'mla_q_norm_g': gain(ks[5], (DEPTH, MLA_Q_RANK)),
        'mla_kv_norm_g': gain(ks[6], (DEPTH, MLA_KV_RANK)),
        'mla_w_uq': nrm(ks[7], (DEPTH, MLA_Q_RANK, MLA_HEADS * (MLA_NOPE + MLA_ROPE)), MLA_Q_RANK ** -0.5),
        'mla_w_ukv': nrm(ks[8], (DEPTH, MLA_KV_RANK, MLA_HEADS * (MLA_NOPE + MLA_V)), MLA_KV_RANK ** -0.5),
        'diff_lambda': nrm(ks[9], (DEPTH, 4, DIFF_DIM), 0.1),
        'diff_norm_g': gain(ks[10], (DEPTH, 2 * DIFF_DIM)),
        'sgu_ln_g': gain(ks[11], (DEPTH, SGU_WIDTH)),
        'sgu_ln_b': nrm(ks[12], (DEPTH, SGU_WIDTH), 0.02),
        'sgu_w': nrm(ks[13], (DEPTH, SGU_GROUPS, SGU_CHUNK, SGU_CHUNK), SGU_CHUNK ** -0.5),
        'sgu_b': gain(ks[14], (DEPTH, SGU_GROUPS, SGU_CHUNK)),
        'mem_norm_g': gain(ks[15], (D_MODEL,)),
        'mem_w_kv': nrm(ks[16], (DEPTH, D_MODEL, 2 * MEM_HEADS * MEM_DIM), D_MODEL ** -0.5),
        'w_branch': nrm(ks[17], (DEPTH, N_BRANCH, BRANCH_WIDTH, D_MODEL), BRANCH_WIDTH ** -0.5),
        'w_out': nrm(ks[18], (DEPTH, D_MODEL, D_MODEL), D_MODEL ** -0.5),
        'final_norm_g': gain(ks[19], (D_MODEL,)),
    }


def reference(x, mem, positions, norm_g, w_in, mla_q_norm_g, mla_kv_norm_g, mla_w_uq, mla_w_ukv,
              diff_lambda, diff_norm_g, sgu_ln_g, sgu_ln_b, sgu_w, sgu_b, mem_norm_g, mem_w_kv,
              w_branch, w_out, final_norm_g):
    B, S, _ = x.shape
    offsets = np.cumsum(SPLIT_SIZES)[:-1].tolist()
    cos_p, sin_p = rope_tables(positions, PARTIAL_ROT)
    cos_m, sin_m = rope_tables(positions, MLA_ROPE)
    mem_n = rms_norm(mem, mem_norm_g)
    h = x
    for l in range(DEPTH):
        xn = rms_norm(h, norm_g[l])
        (a_q, a_k, a_v, i_q, i_k, i_w, b_cq, b_ckv, b_kr, c_q, c_k, c_v,
         d_u, d_v, e_q, gates, merge) = jnp.split(xn @ w_in[l], offsets, axis=-1)
        branches = (
            dsa_branch(a_q, a_k, a_v, i_q, i_k, i_w, cos_p, sin_p),
            mla_branch(b_cq, b_ckv, b_kr, mla_q_norm_g[l], mla_kv_norm_g[l], mla_w_uq[l], mla_w_ukv[l], cos_m, sin_m),
            diff_branch(c_q, c_k, c_v, diff_lambda[l], diff_norm_g[l], l, cos_p, sin_p),
            sgu_branch(d_u, d_v, sgu_ln_g[l], sgu_ln_b[l], sgu_w[l], sgu_b[l]),
            memory_branch(e_q, mem_n, mem_w_kv[l]),
        )
        gates = gates.reshape(B, S, N_BRANCH, BRANCH_WIDTH)
        merge = merge.reshape(B, S, N_BRANCH, D_MODEL)
        mixed = functools.reduce(jnp.add, [
            jax.nn.sigmoid(merge[:, :, n]) * ((o * jax.nn.silu(gates[:, :, n])) @ w_branch[l, n])
            for n, o in enumerate(branches)])
        h = h + mixed @ w_out[l]
    return rms_norm(h, final_norm_g)
```

```python
import math
from contextlib import ExitStack
import numpy as np
import ml_dtypes
import concourse.bass as bass
import concourse.mybir as mybir
from concourse.bass_utils import run_bass_kernel_spmd

F32 = mybir.dt.float32
BF16 = mybir.dt.bfloat16
I32 = mybir.dt.int32
ALU = mybir.AluOpType
AF = mybir.ActivationFunctionType

D = 1024
NCORE = 8
THETA = 500000.0
NEG = -30000.0
O_AQ, O_AK, O_AV, O_IQ, O_IK, O_IW = 0, 512, 576, 640, 896, 960
O_CQL, O_CKV, O_KR = 964, 1348, 1604
O_CQ, O_CK, O_CV = 1636, 2148, 2660
O_DU, O_DV, O_EQ, O_G, O_M = 3172, 3684, 4196, 4708, 7268
IN_W = 12388
Q_A, Q_I, Q_B, Q_C, Q_E, Q_S, Q_G, Q_M, NQ = 0, 512, 768, 1792, 2304, 2816, 3328, 5888, 11008
X_KA, X_KI, X_KB, X_KR, X_KC, X_V, RX = 0, 128, 256, 768, 800, 1312, 2400
V_A, V_B, V_C, VW = 0, 64, 576, 1088


class Buf:
    __slots__ = ("w", "r")

    def __init__(self):
        self.w = None
        self.r = []


class Ctx:
    ENGS = ("tensor", "vector", "scalar", "gpsimd", "sync")

    def __init__(self, nc, stack, n_dma_sems=40):
        self.nc = nc
        self.eng, self.sem, self.cnt, self.waited = {}, {}, {}, {}
        for e in self.ENGS:
            self.eng[e] = getattr(nc, e)
            self.sem[e] = stack.enter_context(nc.semaphore("p_" + e))
            self.cnt[e] = 0
            self.waited[e] = {}
        self.dsems = [stack.enter_context(nc.semaphore("d%d" % i)) for i in range(n_dma_sems)]
        self.dcum = [0] * n_dma_sems
        self.dnext = 0
        self.n_ins = 0

    def _wait(self, e, tok):
        sem, val, src = tok
        key = id(sem)
        w = self.waited[e]
        if w.get(key, 0) >= val:
            return
        w[key] = val
        self.eng[e].wait_ge(sem, val)

    def _deps(self, e, reads, writes):
        for b in reads:
            if b.w is not None:
                self._wait(e, b.w)
        for b in writes:
            if b.w is not None:
                self._wait(e, b.w)
            for t in b.r:
                self._wait(e, t)

    def _commit(self, tok, reads, writes):
        for b in writes:
            b.w = tok
            b.r = []
        for b in reads:
            b.r.append(tok)
            if len(b.r) > 48:
                best = {}
                for t in b.r:
                    k = id(t[0])
                    if k not in best or best[k][1] < t[1]:
                        best[k] = t
                b.r = list(best.values())

    def op(self, e, fn, reads=(), writes=()):
        self._deps(e, reads, writes)
        ins = fn(self.eng[e])
        self.cnt[e] += 1
        ins.then_inc(self.sem[e], 1)
        tok = (self.sem[e], self.cnt[e], e)
        self.waited[e][id(self.sem[e])] = max(self.waited[e].get(id(self.sem[e]), 0), 0)
        self._commit(tok, reads, writes)
        self.n_ins += 1
        return tok

    def dma(self, q, out, in_, reads=(), writes=()):
        i = self.dnext
        self.dnext = (self.dnext + 1) % len(self.dsems)
        sem = self.dsems[i]
        if self.dcum[i] > 0:
            self._wait(q, (sem, self.dcum[i], None))
        self._deps(q, reads, writes)
        ins = self.eng[q].dma_start(out=out, in_=in_)
        self.dcum[i] += 16
        ins.then_inc(sem, 16)
        tok = (sem, self.dcum[i], None)
        self._commit(tok, reads, writes)
        self.n_ins += 1
        return tok

    def all_tokens(self):
        toks = [(self.sem[e], self.cnt[e], e) for e in self.ENGS if self.cnt[e] > 0]
        toks += [(self.dsems[i], self.dcum[i], None) for i in range(len(self.dsems)) if self.dcum[i] > 0]
        return toks

    def barrier(self, engines=None):
        toks = self.all_tokens()
        for e in (engines or self.ENGS):
            for t in toks:
                if t[2] != e:
                    self._wait(e, t)


class Ring:
    def __init__(self, tensors):
        self.t = tensors
        self.b = [Buf() for _ in tensors]
        self.i = 0

    def next(self):
        k = self.i
        self.i = (self.i + 1) % len(self.t)
        return self.t[k], self.b[k]


def _fm_units():
    units = []

    def partner_P(cols):
        p = np.full(128, -1, np.int64)
        for r in range(128):
            j = r % 64
            if j < 8:
                p[r] = cols[r + 8]
            elif j < 16:
                p[r] = cols[r - 8]
        return p

    ar = np.arange(128)
    for t in range(4):
        cols = O_AQ + 128 * t + ar
        units.append(("qA%d" % t, cols, partner_P(cols), "P", ("Q", Q_A + 128 * t)))
    cols = O_AK + (ar % 64)
    units.append(("kA", cols, partner_P(cols), "P", ("X", X_KA, 0, 128)))
    for t in range(2):
        cols = O_IQ + 128 * t + ar
        units.append(("qI%d" % t, cols, partner_P(cols), "P", ("Q", Q_I + 128 * t)))
    cols = O_IK + (ar % 64)
    units.append(("kI", cols, partner_P(cols), "P", ("X", X_KI, 0, 128)))
    for t in range(3):
        units.append(("cq%d" % t, O_CQL + 128 * t + ar, None, None, ("CQ", t)))
    for t in range(2):
        units.append(("ckv%d" % t, O_CKV + 128 * t + ar, None, None, ("CKV", t)))
    cols = np.full(128, -1, np.int64)
    cols[64:96] = O_KR + np.arange(32)
    p = np.full(128, -1, np.int64)
    p[64:80] = cols[80:96]
    p[80:96] = cols[64:80]
    units.append(("kr", cols, p, "M", ("X", X_KR, 64, 32)))
    for t in range(4):
        cols = O_CQ + 128 * t + ar
        units.append(("qC%d" % t, cols, partner_P(cols), "P", ("Q", Q_C + 128 * t)))
    for t in range(4):
        cols = O_CK + 128 * t + ar
        units.append(("kC%d" % t, cols, partner_P(cols), "P", ("X", X_KC + 128 * t, 0, 128)))
    for t in range(4):
        units.append(("dU%d" % t, O_DU + 128 * t + ar, None, None, ("DU", t)))
    for t in range(4):
        units.append(("eQ%d" % t, O_EQ + 128 * t + ar, None, None, ("Q", Q_E + 128 * t)))
    for t in range(20):
        units.append(("g%d" % t, O_G + 128 * t + ar, None, None, ("Q", Q_G + 128 * t)))
    for t in range(40):
        units.append(("m%d" % t, O_M + 128 * t + ar, None, None, ("Q", Q_M + 128 * t)))
    return units


FM_UNITS = _fm_units()
N_FM_TILES = sum(1 + (u[2] is not None) for u in FM_UNITS)
TM_COLS = np.concatenate([O_CV + np.arange(512), O_DV + np.arange(512), O_AV + np.arange(64), O_IW + np.arange(4),
                          np.full(60, -1, np.int64)])
N_W_TILES = N_FM_TILES + 9
NCOL = N_W_TILES * 128


def _win_index():
    idx = []
    for u in FM_UNITS:
        idx.append(u[1])
        if u[2] is not None:
            idx.append(u[2])
    idx.append(TM_COLS)
    idx = np.concatenate(idx)
    return np.where(idx < 0, IN_W, idx)


def _uq_index():
    idx = []
    for hq in range(8):
        cols = np.full(128, -1, np.int64)
        cols[:96] = hq * 96 + np.arange(96)
        idx.append(cols)
    for hq in range(8):
        cols = np.full(128, -1, np.int64)
        cols[64:80] = hq * 96 + 80 + np.arange(16)
        cols[80:96] = hq * 96 + 64 + np.arange(16)
        idx.append(cols)
    idx = np.concatenate(idx)
    return np.where(idx < 0, 768, idx)


def _ukv_index():
    k = np.concatenate([h * 128 + np.arange(64) for h in range(8)])
    v = np.concatenate([h * 128 + 64 + np.arange(64) for h in range(8)])
    return np.concatenate([k, v])


def _rope_consts():
    rc = np.zeros((128, 4), np.float32)
    for r in range(128):
        j = r % 64
        if j < 16:
            rc[r, 0] = np.float32(THETA) ** np.float32(-(2.0 * (j % 8)) / 16.0)
            rc[r, 1] = -1.0 if j < 8 else 1.0
        if 64 <= r < 96:
            jj = r - 64
            rc[r, 2] = np.float32(THETA) ** np.float32(-(2.0 * (jj % 16)) / 32.0)
            rc[r, 3] = -1.0 if jj < 16 else 1.0
    return rc


def build_program(S, depth, debug=False):
    NSB = S // 512
    NL = NSB // NCORE
    NSLOT = 2 * NL
    T = NSLOT * 512
    NKMAX = 32 * NL
    nc = bass.Bass("TRN2", target_bir_lowering=False)

    def din(name, shape, dt=F32):
        return nc.dram_tensor(name, list(shape), dt, kind="ExternalInput").ap()

    def dscr(name, shape, dt):
        return nc.dram_tensor(name, list(shape), dt).ap()

    x_in = din("x", [T, D])
    pos_in = din("pos", [T], I32)
    mem_in = din("mem", [2, 256, D])
    cidx_in = din("cidx", [128, 1])
    ropec_in = din("ropec", [128, 4])
    ng_in = din("norm_g", [depth, 128, 8])
    win_in = din("w_in", [depth, D, NCOL])
    qg_in = din("qg", [depth, 128, 3])
    kvg_in = din("kvg", [depth, 128, 2])
    wuq_in = din("w_uq", [depth, 384, 2048])
    wukv_in = din("w_ukv", [depth, 256, 1024])
    dlam_in = din("dlam", [depth, 4, 64])
    dng_in = din("dng", [depth, 128, 1])
    slg_in = din("sgu_g", [depth, 512])
    slb_in = din("sgu_b", [depth, 512])
    swt_in = din("sgu_wT", [depth, 8, 128, 128])
    sbs_in = din("sgu_bs", [depth, 8, 128])
    mng_in = din("mem_g", [D])
    wkv_in = din("w_kv", [depth, D, 1024])
    wbr_in = din("w_br", [depth, 5, 512, D])
    wout_in = din("w_out", [depth, D, D])
    fng_in = din("fin_g", [D])
    y_out = nc.dram_tensor("y", [T, D], F32, kind="ExternalOutput").ap()

    hbuf = dscr("hbuf", [T, D], F32)
    wbf = dscr("wbf", [N_W_TILES, 128, 8 * 128], BF16)
    wuqb = dscr("wuqb", [16, 128, 3 * 128], BF16)
    wukb = dscr("wukb", [4, 128, 2 * 128], BF16)
    wuvb = dscr("wuvb", [128, 2 * 512], BF16)
    wkvb = dscr("wkvb", [128, 8 * 1024], BF16)
    wbrb = dscr("wbrb", [5, 128, 4 * 1024], BF16)
    woutb = dscr("woutb", [128, 8 * 1024], BF16)
    QS = dscr("QS", [NQ, T], BF16)
    iwtm = dscr("iwtm", [T, 4], F32)
    iwT = dscr("iwT", [4, T], F32)
    XL = dscr("XL", [NSLOT * RX, 512], BF16)
    XG = dscr("XG", [NCORE * NSLOT * RX, 512], BF16)
    mbuf = dscr("mbuf", [128, NKMAX * 512], BF16)
    ropeT = dscr("ropeT", [4, 128, T], F32)
    dbg = {}
    if debug:
        dbg["QS"] = nc.dram_tensor("dbg_QS", [NQ, T], BF16, kind="ExternalOutput").ap()
        dbg["XL"] = nc.dram_tensor("dbg_XL", [NSLOT * RX, 512], BF16, kind="ExternalOutput").ap()
        dbg["OB"] = nc.dram_tensor("dbg_OB", [5, 512, T], F32, kind="ExternalOutput").ap()
        dbg["TH"] = nc.dram_tensor("dbg_TH", [T, 1], F32, kind="ExternalOutput").ap()

    def xg_rows(r, slot, row0, n):
        base = (r * NSLOT + slot) * RX + row0
        return XG[base:base + n, :]

    def vview(buf, base):
        return buf[base:base + 1088, :].rearrange("r c -> (r c)").rearrange("(t p k) -> p t k", t=4, p=128)

    with ExitStack() as st:
        c = Ctx(nc, st)

        cur = [st]
        uid = [0]

        def sb(name, shape, dt):
            uid[0] += 1
            return cur[0].enter_context(nc.sbuf_tensor("%s_%d" % (name, uid[0]), list(shape), dt))

        V = lambda fn, r=(), w=(): c.op("vector", fn, r, w)
        G = lambda fn, r=(), w=(): c.op("gpsimd", fn, r, w)
        A = lambda fn, r=(), w=(): c.op("scalar", fn, r, w)
        P = lambda fn, r=(), w=(): c.op("tensor", fn, r, w)
        LD = lambda out, in_, r=(), w=(): c.dma("sync", out, in_, r, w)
        STO = lambda out, in_, r=(), w=(): c.dma("gpsimd", out, in_, r, w)

        def RSQ(o_ap, i_ap, rb, wb):
            A(lambda e: e.activation(out=o_ap, in_=i_ap, func=AF.Ln), r=rb, w=wb)
            A(lambda e: e.activation(out=o_ap, in_=o_ap, func=AF.Exp, scale=-0.5), r=wb, w=wb)

        PS = [st.enter_context(nc.psum_tensor("ps%d" % i, [128, 512], F32)) for i in range(8)]
        bPS = [Buf() for _ in range(8)]
        psr = Ring(PS[0:3]); psr.b = bPS[0:3]
        ident_f = sb("ident_f", [128, 128], F32)
        ident_b = sb("ident_b", [128, 128], BF16)
        ones_f = sb("ones_f", [128, 128], F32)
        ones_b = sb("ones_b", [128, 128], BF16)
        stripT = sb("stripT", [128, 35 * 128], BF16)
        stripQ = sb("stripQ", [128, 35 * 128], BF16)
        trilT = sb("trilT", [128, 128], F32)
        cidx = sb("cidx", [128, 1], F32)
        ropec = sb("ropec", [128, 4], F32)
        bconst = Buf()
        stage = sb("stage", [128, 4096], F32)
        bstage = Buf()
        memT = sb("memT", [128, 2, 8, 256], BF16)
        bmemT = Buf()
        gbc = sb("gbc", [128, 1024], F32)
        bgbc = Buf()
        small = sb("small", [128, 64], F32)
        bsmall = Buf()
        junk = sb("junk", [128, 1024], BF16)
        bjunk = Buf()
        t1_ring = Ring([sb("t1_%d" % i, [128, 512], F32) for i in range(3)])
        dvf = sb("dvf", [128, 512], F32)
        bdvf = Buf()
        selT = sb("selT", [4, 4, 128], F32)
        lam = sb("lam", [128, 4], F32)
        blam = Buf()
        dng = sb("dng", [128, 1], F32)
        ngs = sb("ngs", [128, 8], F32)
        qgs = sb("qgs", [128, 3], F32)
        kvgs = sb("kvgs", [128, 2], F32)
        blw = Buf()
        st0 = ExitStack()
        cur[0] = st0
        posi = sb("posi", [128, 512], I32)
        posf = sb("posf", [128, 512], F32)
        ang = sb("ang", [128, 512], F32)
        tabo = sb("tabo", [128, 512], F32)
        iot = sb("iot", [128, 35 * 128], I32)
        iotf = sb("iotf", [128, 35 * 128], F32)
        LD(cidx[:], cidx_in, w=[bconst])
        LD(ropec[:], ropec_in, w=[bconst])
        G(lambda e: e.memset(ones_f[:], 1.0), w=[bconst])
        G(lambda e: e.memset(ones_b[:], 1.0), w=[bconst])
        G(lambda e: e.iota(iot[:, 0:128], [[1, 128]], base=0, channel_multiplier=-1), w=[bconst])
        V(lambda e: e.tensor_copy(out=iotf[:, 0:128], in_=iot[:, 0:128]), r=[bconst], w=[bconst])
        V(lambda e: e.tensor_scalar(out=ident_f[:], in0=iotf[:, 0:128], scalar1=0.0, scalar2=None, op0=ALU.is_equal), r=[bconst], w=[bconst])
        V(lambda e: e.tensor_copy(out=ident_b[:], in_=ident_f[:]), r=[bconst], w=[bconst])
        V(lambda e: e.tensor_scalar(out=trilT[:], in0=iotf[:, 0:128], scalar1=0.0, scalar2=None, op0=ALU.is_ge), r=[bconst], w=[bconst])
        G(lambda e: e.iota(iot[:], [[128, 35], [1, 128]], base=-31 * 128, channel_multiplier=-1), r=[bconst], w=[bconst])
        V(lambda e: e.tensor_copy(out=iotf[:], in_=iot[:]), r=[bconst], w=[bconst])
        V(lambda e: e.tensor_scalar(out=iotf[:], in0=iotf[:], scalar1=cidx[:, 0:1], scalar2=0.0, op0=ALU.add, op1=ALU.is_lt), r=[bconst], w=[bconst])
        V(lambda e: e.tensor_scalar(out=stripT[:], in0=iotf[:], scalar1=NEG, scalar2=None, op0=ALU.mult), r=[bconst], w=[bconst])
        G(lambda e: e.iota(iot[:], [[-128, 35], [-1, 128]], base=3 * 128, channel_multiplier=1), r=[bconst], w=[bconst])
        V(lambda e: e.tensor_copy(out=iotf[:], in_=iot[:]), r=[bconst], w=[bconst])
        V(lambda e: e.tensor_scalar(out=iotf[:], in0=iotf[:], scalar1=cidx[:, 0:1], scalar2=0.0, op0=ALU.add, op1=ALU.is_lt), r=[bconst], w=[bconst])
        V(lambda e: e.tensor_scalar(out=stripQ[:], in0=iotf[:], scalar1=-1.0e30, scalar2=None, op0=ALU.mult), r=[bconst], w=[bconst])


        bropeT = Buf()
        bpos, bang, btab = Buf(), Buf(), Buf()
        TWO_PI = 2.0 * math.pi
        for tt in range(T // 512):
            LD(posi[:], pos_in[tt * 512:(tt + 1) * 512].partition_broadcast(128), w=[bpos])
            V(lambda e: e.tensor_copy(out=posf[:], in_=posi[:]), r=[bpos], w=[bpos])
            for k, (fcol, scol, shift) in enumerate([(0, None, 0.5 * math.pi), (0, 1, 0.0), (2, None, 0.5 * math.pi), (2, 3, 0.0)]):
                V(lambda e: e.tensor_scalar(out=ang[:], in0=posf[:], scalar1=ropec[:, fcol:fcol + 1], scalar2=shift, op0=ALU.mult, op1=ALU.add), r=[bpos, bconst], w=[bang])
                V(lambda e: e.tensor_scalar(out=tabo[:], in0=ang[:], scalar1=1.0 / TWO_PI, scalar2=12582912.0, op0=ALU.mult, op1=ALU.add), r=[bang], w=[btab])
                V(lambda e: e.tensor_scalar(out=tabo[:], in0=tabo[:], scalar1=-12582912.0, scalar2=None, op0=ALU.add), r=[btab], w=[btab])
                V(lambda e: e.scalar_tensor_tensor(out=ang[:], in0=tabo[:], scalar=-TWO_PI, in1=ang[:], op0=ALU.mult, op1=ALU.add), r=[btab, bang], w=[bang])
                V(lambda e: e.tensor_scalar(out=ang[:], in0=ang[:], scalar1=math.pi, scalar2=-math.pi, op0=ALU.min, op1=ALU.max), r=[bang], w=[bang])
                A(lambda e: e.activation(out=tabo[:], in_=ang[:], func=AF.Sin), r=[bang], w=[btab])
                if scol is not None:
                    V(lambda e: e.tensor_scalar(out=tabo[:], in0=tabo[:], scalar1=ropec[:, scol:scol + 1], scalar2=None, op0=ALU.mult), r=[btab, bconst], w=[btab])
                STO(ropeT[k, :, tt * 512:(tt + 1) * 512], tabo[:], r=[btab], w=[bropeT])

        bh = Buf()
        for tt in range(T // 512):
            for half in range(2):
                v = stage[:, 0:2048].rearrange("p (a f) -> p a f", a=2)
                src = x_in[tt * 512 + half * 256: tt * 512 + half * 256 + 256, :].rearrange("(a p) f -> p a f", p=128)
                dst = hbuf[tt * 512 + half * 256: tt * 512 + half * 256 + 256, :].rearrange("(a p) f -> p a f", p=128)
                LD(v, src, w=[bstage])
                STO(dst, v, r=[bstage], w=[bh])

        LD(gbc[:], mng_in.partition_broadcast(128), w=[bgbc])
        for b in range(2):
            for mt in range(2):
                hv = stage[:, 0:1024]
                LD(hv, mem_in[b, mt * 128:(mt + 1) * 128, :], w=[bstage])
                A(lambda e: e.activation(out=junk[:], in_=hv, func=AF.Square, accum_out=small[:, 0:1]), r=[bstage], w=[bjunk, bsmall])
                V(lambda e: e.tensor_scalar(out=small[:, 1:2], in0=small[:, 0:1], scalar1=1.0 / D, scalar2=1e-6, op0=ALU.mult, op1=ALU.add), r=[bsmall], w=[bsmall])
                RSQ(small[:, 1:2], small[:, 1:2], [bsmall], [bsmall])
                V(lambda e: e.scalar_tensor_tensor(out=hv, in0=hv, scalar=small[:, 1:2], in1=gbc[:], op0=ALU.mult, op1=ALU.mult), r=[bstage, bsmall, bgbc], w=[bstage])
                for kh in range(2):
                    pt, bpt = psr.next()
                    for q in range(4):
                        kc = kh * 4 + q
                        P(lambda e: e.transpose(out=pt[:, q * 128:(q + 1) * 128], in_=hv[:, kc * 128:(kc + 1) * 128], identity=ident_f[:]), r=[bstage, bconst], w=[bpt])
                    V(lambda e: e.tensor_copy(out=memT[:, b, kh * 4:(kh + 1) * 4, mt * 128:(mt + 1) * 128],
                                              in_=pt[:].rearrange("p (q f) -> p q f", q=4)), r=[bpt], w=[bmemT])

        c.barrier()
        st0.close()
        cur[0] = st

        bwbf, bwsm_d, bXL, bXG, bQS, biw = Buf(), Buf(), Buf(), Buf(), Buf(), Buf()
        ccs = st.enter_context(nc.semaphore("ccsem"))
        cc_count = [0]

        def convert(dst_ap, src_ap, n, scale_ap=None):
            done = 0
            while done < n:
                m = min(4096, n - done)
                sv = stage[:, 0:m]
                bv = stage[:, 4096:4096 + m // 2].bitcast(BF16) if False else None
                LD(sv, src_ap[:, done:done + m], w=[bstage])
                ov = junk[:, 0:1024]
                for j in range(0, m, 1024):
                    mm = min(1024, m - j)
                    if scale_ap is not None:
                        V(lambda e: e.tensor_scalar(out=junk[:, 0:mm], in0=stage[:, j:j + mm], scalar1=scale_ap, scalar2=None, op0=ALU.mult), r=[bstage, blw], w=[bjunk])
                    else:
                        V(lambda e: e.tensor_copy(out=junk[:, 0:mm], in_=stage[:, j:j + mm]), r=[bstage], w=[bjunk])
                    STO(dst_ap[:, done + j:done + j + mm], junk[:, 0:mm], r=[bjunk], w=[bwbf])
                done += m

        def prep_layer(l):
            lam_init = 0.8 - 0.6 * math.exp(-0.3 * l)
            LD(ngs[:], ng_in[l], w=[blw])
            LD(qgs[:], qg_in[l], w=[blw])
            LD(kvgs[:], kvg_in[l], w=[blw])
            LD(dng[:], dng_in[l], w=[blw])
            for t in range(N_W_TILES):
                for kc in range(8):
                    pass
            for t0 in range(0, N_W_TILES, 4):
                nt = min(4, N_W_TILES - t0)
                for kc in range(8):
                    sv = stage[:, kc * 512: kc * 512 + nt * 128]
                    LD(sv, win_in[l, kc * 128:(kc + 1) * 128, t0 * 128:(t0 + nt) * 128], w=[bstage])
                for kc in range(8):
                    V(lambda e: e.tensor_scalar(out=junk[:, kc * 128 * 0: 0 + nt * 128] if False else stage[:, kc * 512: kc * 512 + nt * 128],
                                                in0=stage[:, kc * 512: kc * 512 + nt * 128], scalar1=ngs[:, kc:kc + 1], scalar2=None, op0=ALU.mult),
                      r=[bstage, blw], w=[bstage])
                for tq in range(nt):
                    src = stage[:, 0:4096].rearrange("p (k f) -> p k f", k=8)[:, :, tq * 128:(tq + 1) * 128]
                    ov = junk[:, 0:1024].rearrange("p (k f) -> p k f", k=8)
                    V(lambda e: e.tensor_copy(out=ov, in_=src), r=[bstage], w=[bjunk])
                    STO(wbf[t0 + tq], junk[:, 0:1024], r=[bjunk], w=[bwbf])
            for t0 in range(0, 16, 8):
                for kc in range(3):
                    LD(stage[:, kc * 1024:(kc + 1) * 1024], wuq_in[l, kc * 128:(kc + 1) * 128, t0 * 128:(t0 + 8) * 128], w=[bstage])
                    V(lambda e: e.tensor_scalar(out=stage[:, kc * 1024:(kc + 1) * 1024], in0=stage[:, kc * 1024:(kc + 1) * 1024], scalar1=qgs[:, kc:kc + 1], scalar2=None, op0=ALU.mult), r=[bstage, blw], w=[bstage])
                for tq in range(8):
                    src = stage[:, 0:3072].rearrange("p (k f) -> p k f", k=3)[:, :, tq * 128:(tq + 1) * 128]
                    ov = junk[:, 0:384].rearrange("p (k f) -> p k f", k=3)
                    V(lambda e: e.tensor_copy(out=ov, in_=src), r=[bstage], w=[bjunk])
                    STO(wuqb[t0 + tq], junk[:, 0:384], r=[bjunk], w=[bwbf])
            for kc in range(2):
                LD(stage[:, kc * 1024:(kc + 1) * 1024], wukv_in[l, kc * 128:(kc + 1) * 128, :], w=[bstage])
                V(lambda e: e.tensor_scalar(out=stage[:, kc * 1024:(kc + 1) * 1024], in0=stage[:, kc * 1024:(kc + 1) * 1024], scalar1=kvgs[:, kc:kc + 1], scalar2=None, op0=ALU.mult), r=[bstage, blw], w=[bstage])
            for tq in range(4):
                src = stage[:, 0:2048].rearrange("p (k f) -> p k f", k=2)[:, :, tq * 128:(tq + 1) * 128]
                ov = junk[:, 0:256].rearrange("p (k f) -> p k f", k=2)
                V(lambda e: e.tensor_copy(out=ov, in_=src), r=[bstage], w=[bjunk])
                STO(wukb[tq], junk[:, 0:256], r=[bjunk], w=[bwbf])
            src = stage[:, 0:2048].rearrange("p (k f) -> p k f", k=2)[:, :, 512:1024]
            ov = junk[:, 0:1024].rearrange("p (k f) -> p k f", k=2)
            V(lambda e: e.tensor_copy(out=ov, in_=src), r=[bstage], w=[bjunk])
            STO(wuvb[:, :], junk[:, 0:1024], r=[bjunk], w=[bwbf])
            for kc in range(8):
                convert(wkvb[:, kc * 1024:(kc + 1) * 1024], wkv_in[l, kc * 128:(kc + 1) * 128, :], 1024)
                convert(woutb[:, kc * 1024:(kc + 1) * 1024], wout_in[l, kc * 128:(kc + 1) * 128, :], 1024)
            for n in range(5):
                for kc in range(4):
                    convert(wbrb[n][:, kc * 1024:(kc + 1) * 1024], wbr_in[l, n, kc * 128:(kc + 1) * 128, :], 1024)
            LD(wuv[:], wuvb[:, :], r=[bwbf], w=[bwuv])
            for kc in range(8):
                pass
            for t9 in range(9):
                LD(wtm[:, :, t9 * 128:(t9 + 1) * 128], wbf[N_FM_TILES + t9].rearrange("p (k f) -> p k f", k=8), r=[bwbf], w=[bwtm])
            LD(lnp[:, 0, :], slg_in[l].partition_broadcast(128), w=[blnp])
            LD(lnp[:, 1, :], slb_in[l].partition_broadcast(128), w=[blnp])
            LD(stage[:, 0:1024].rearrange("p (g f) -> p g f", g=8), swt_in[l].rearrange("g j i -> j g i"), w=[bstage])
            for g in range(8):
                V(lambda e: e.tensor_tensor(out=wcT[:, g, :], in0=stage[:, g * 128:(g + 1) * 128], in1=trilT[:], op=ALU.mult), r=[bstage, bconst], w=[bwcT])
            for g in range(8):
                LD(bsT[(g % 2) * 64:(g % 2) * 64 + 64, g // 2, :], sbs_in[l, g].partition_broadcast(64), w=[bbsT])
            LD(stage[0:1, 0:256], dlam_in[l].rearrange("a f -> (a f)").partition_broadcast(1), w=[bstage])
            V(lambda e: e.tensor_tensor(out=stage[0:1, 256:384].rearrange("p (a f) -> p a f", a=2),
                                        in0=stage[0:1, 0:256].rearrange("p (a b f) -> p a b f", a=2, b=2)[:, :, 0, :],
                                        in1=stage[0:1, 0:256].rearrange("p (a b f) -> p a b f", a=2, b=2)[:, :, 1, :], op=ALU.mult), r=[bstage], w=[bstage])
            V(lambda e: e.tensor_reduce(out=stage[0:1, 384:386], in_=stage[0:1, 256:384].rearrange("p (a f) -> p a f", a=2), axis=mybir.AxisListType.X, op=ALU.add), r=[bstage], w=[bstage])
            V(lambda e: e.memset(stage[0:1, 386:392], 0.0), w=[bstage])
            A(lambda e: e.activation(out=stage[0:1, 386:388], in_=stage[0:1, 384:386], func=AF.Exp), r=[bstage], w=[bstage])
            V(lambda e: e.tensor_tensor(out=stage[0:1, 388:390], in0=stage[0:1, 386:388], in1=stage[0:1, 387:389], op=ALU.subtract), r=[bstage], w=[bstage])
            V(lambda e: e.tensor_scalar(out=stage[0:1, 390:392], in0=stage[0:1, 388:390], scalar1=lam_init, scalar2=-1.0, op0=ALU.add, op1=ALU.mult), r=[bstage], w=[bstage])
            pt, bpt = psr.next()
            P(lambda e: e.matmul(pt[:, 0:2], lhsT=ones_f[0:1, :], rhs=stage[0:1, 390:392], start=True, stop=True), r=[bstage, bconst], w=[bpt])
            V(lambda e: e.tensor_copy(out=lam[:, 1:2], in_=pt[:, 0:1]), r=[bpt], w=[blam])
            return lam_init

        def phase1(l, tt):
            slot = tt
            hb = stage[:, 0:4096].rearrange("p (m f) -> p m f", m=4)
            LD(hb, hbuf[tt * 512:(tt + 1) * 512, :].rearrange("(m p) f -> p m f", p=128), r=[bh], w=[bstage])
            for m in range(4):
                A(lambda e: e.activation(out=junk[:], in_=hb[:, m, :], func=AF.Square, accum_out=small[:, m:m + 1]), r=[bstage], w=[bjunk, bsmall])
            V(lambda e: e.tensor_scalar(out=small[:, 4:8], in0=small[:, 0:4], scalar1=1.0 / D, scalar2=1e-6, op0=ALU.mult, op1=ALU.add), r=[bsmall], w=[bsmall])
            RSQ(rstd[:], small[:, 4:8], [bsmall], [brstd])
            for m in range(4):
                for kh in range(2):
                    pt, bpt = psr.next()
                    for q in range(4):
                        kc = kh * 4 + q
                        P(lambda e: e.transpose(out=pt[:, q * 128:(q + 1) * 128], in_=hb[:, m, kc * 128:(kc + 1) * 128], identity=ident_f[:]), r=[bstage, bconst], w=[bpt])
                    V(lambda e: e.tensor_copy(out=hT[:, kh * 4:(kh + 1) * 4, m * 128:(m + 1) * 128], in_=pt[:].rearrange("p (q f) -> p q f", q=4)), r=[bpt], w=[bhT])
            prb, bprb = psr.next()
            for m in range(4):
                V(lambda e: e.tensor_scalar(out=dvf[:, m * 128:(m + 1) * 128], in0=ident_f[:], scalar1=rstd[:, m:m + 1], scalar2=None, op0=ALU.mult), r=[bconst, brstd], w=[bdvf])
            for m in range(4):
                P(lambda e: e.matmul(prb[:, m * 128:(m + 1) * 128], lhsT=ones_f[:], rhs=dvf[:, m * 128:(m + 1) * 128], start=True, stop=True), r=[bdvf, bconst], w=[bprb])
            V(lambda e: e.tensor_copy(out=rbc[:], in_=prb[:]), r=[bprb], w=[brbc])
            LD(tabs[:], ropeT[:, :, tt * 512:(tt + 1) * 512].rearrange("k p f -> p k f"), r=[bropeT], w=[btabs])
            for k in range(2):
                V(lambda e: e.tensor_tensor(out=tabs[:, k, :], in0=tabs[:, k, :], in1=rbc[:], op=ALU.mult), r=[btabs, brbc], w=[btabs])

            def proj(wtile_idx, nk, wsrc, rhs_fn, ring=wt_ring):
                wt, bwt = ring.next()
                LD(wt[:, 0:nk * 128], wsrc, r=[bwbf], w=[bwt])
                pt, bpt = psr.next()
                for kc in range(nk):
                    rhs, rb = rhs_fn(kc)
                    P(lambda e: e.matmul(pt[:], lhsT=wt[:, kc * 128:(kc + 1) * 128], rhs=rhs, start=(kc == 0), stop=(kc == nk - 1)), r=[bwt] + rb, w=[bpt])
                return pt, bpt

            def rope_out(pm, bpm, pp, bpp, ci, si, scaled):
                t1, bt1 = t1_ring.next()
                ev, bev = ev_ring.next()
                V(lambda e: e.tensor_tensor(out=t1[:], in0=pm[:], in1=tabs[:, ci, :], op=ALU.mult), r=[bpm, btabs], w=[bt1])
                V(lambda e: e.tensor_tensor(out=ev[:], in0=pp[:], in1=tabs[:, si, :], op=ALU.mult), r=[bpp, btabs], w=[bev])
                G(lambda e: e.tensor_tensor(out=ev[:], in0=ev[:], in1=t1[:], op=ALU.add), r=[bt1, bev], w=[bev])
                return ev, bev

            hrhs = lambda kc: (hT[:, kc, :], [bhT])
            ti = 0
            for (name, cols, partner, rope, dest) in FM_UNITS:
                pm, bpm = proj(ti, 8, wbf[ti], hrhs)
                ti += 1
                if partner is not None:
                    pp, bpp = proj(ti, 8, wbf[ti], hrhs)
                    ti += 1
                    if rope == "P":
                        ev, bev = rope_out(pm, bpm, pp, bpp, 0, 1, True)
                    else:
                        t1, bt1 = t1_ring.next()
                        t2, bt2 = t1_ring.next()
                        ev, bev = ev_ring.next()
                        V(lambda e: e.tensor_tensor(out=t1[:], in0=pm[:], in1=tabs[:, 2, :], op=ALU.mult), r=[bpm, btabs], w=[bt1])
                        V(lambda e: e.tensor_tensor(out=t2[:], in0=pp[:], in1=tabs[:, 3, :], op=ALU.mult), r=[bpp, btabs], w=[bt2])
                        G(lambda e: e.tensor_tensor(out=t1[:], in0=t1[:], in1=t2[:], op=ALU.add), r=[bt1, bt2], w=[bt1])
                        V(lambda e: e.tensor_tensor(out=ev[:], in0=t1[:], in1=rbc[:], op=ALU.mult), r=[bt1, brbc], w=[bev])
                else:
                    if dest[0] == "CQ":
                        V(lambda e: e.tensor_tensor(out=cqf[:, dest[1], :], in0=pm[:], in1=rbc[:], op=ALU.mult), r=[bpm, brbc], w=[bcqf])
                        if dest[1] == 2:
                            mla_q(tt)
                        continue
                    if dest[0] == "CKV":
                        V(lambda e: e.tensor_tensor(out=cqf[:, dest[1], :], in0=pm[:], in1=rbc[:], op=ALU.mult), r=[bpm, brbc], w=[bcqf])
                        if dest[1] == 1:
                            latent_norm(2, 256.0)
                            mla_kv(slot)
                        continue
                    ev, bev = ev_ring.next()
                    if dest[0] == "DU":
                        V(lambda e: e.tensor_tensor(out=dUs[:, dest[1], :], in0=pm[:], in1=rbc[:], op=ALU.mult), r=[bpm, brbc], w=[bdU])
                        continue
                    V(lambda e: e.tensor_tensor(out=ev[:], in0=pm[:], in1=rbc[:], op=ALU.mult), r=[bpm, brbc], w=[bev])
                if dest[0] == "Q":
                    STO(QS[dest[1]:dest[1] + 128, tt * 512:(tt + 1) * 512], ev[:], r=[bev], w=[bQS])
                else:
                    _, row0, p0, n = dest
                    STO(XL[slot * RX + row0: slot * RX + row0 + n, :], ev[p0:p0 + n, :], r=[bev], w=[bXL])
                if name == "cq2":
                    pass
                if dest[0] == "X" and name == "kr":
                    pass
                if name == "kI":
                    pass
            return

        def latent_norm(nk, width):
            pt, bpt = psr.next()
            for kc in range(nk):
                t1, bt1 = t1_ring.next()
                V(lambda e: e.tensor_tensor(out=t1[:], in0=cqf[:, kc, :], in1=cqf[:, kc, :], op=ALU.mult), r=[bcqf], w=[bt1])
                P(lambda e: e.matmul(pt[:], lhsT=ones_f[:], rhs=t1[:], start=(kc == 0), stop=(kc == nk - 1)), r=[bt1, bconst], w=[bpt])
            t1, bt1 = t1_ring.next()
            V(lambda e: e.tensor_scalar(out=t1[:], in0=pt[:], scalar1=1.0 / width, scalar2=1e-6, op0=ALU.mult, op1=ALU.add), r=[bpt], w=[bt1])
            RSQ(t1[:], t1[:], [bt1], [bt1])
            for kc in range(nk):
                V(lambda e: e.tensor_tensor(out=cqn[:, kc, :], in0=cqf[:, kc, :], in1=t1[:], op=ALU.mult), r=[bcqf, bt1], w=[bcqn])

        def small_proj(wsrc, nk):
            wt, bwt = wsm_ring.next()
            LD(wt[:, 0:nk * 128], wsrc, r=[bwbf], w=[bwt])
            pt, bpt = psr.next()
            for kc in range(nk):
                P(lambda e: e.matmul(pt[:], lhsT=wt[:, kc * 128:(kc + 1) * 128], rhs=cqn[:, kc, :], start=(kc == 0), stop=(kc == nk - 1)), r=[bwt, bcqn], w=[bpt])
            return pt, bpt

        def mla_q(tt):
            latent_norm(3, 384.0)
            for hq in range(8):
                pm, bpm = small_proj(wuqb[hq], 3)
                pp, bpp = small_proj(wuqb[8 + hq], 3)
                t1, bt1 = t1_ring.next()
                ev, bev = ev_ring.next()
                V(lambda e: e.tensor_tensor(out=t1[:], in0=pm[:], in1=tabs[:, 2, :], op=ALU.mult), r=[bpm, btabs], w=[bt1])
                V(lambda e: e.tensor_tensor(out=ev[:], in0=pp[:], in1=tabs[:, 3, :], op=ALU.mult), r=[bpp, btabs], w=[bev])
                G(lambda e: e.tensor_tensor(out=ev[:], in0=ev[:], in1=t1[:], op=ALU.add), r=[bt1, bev], w=[bev])
                STO(QS[Q_B + 128 * hq: Q_B + 128 * hq + 128, tt * 512:(tt + 1) * 512], ev[:], r=[bev], w=[bQS])

        def mla_kv(slot):
            for tq in range(4):
                pm, bpm = small_proj(wukb[tq], 2)
                ev, bev = ev_ring.next()
                A(lambda e: e.activation(out=ev[:], in_=pm[:], func=AF.Copy), r=[bpm], w=[bev])
                STO(XL[slot * RX + X_KB + 128 * tq: slot * RX + X_KB + 128 * tq + 128, :], ev[:], r=[bev], w=[bXL])
            xv = vview(XL, slot * RX + X_V)
            for m in range(4):
                pt, bpt = psr.next()
                for kc in range(2):
                    P(lambda e: e.matmul(pt[:], lhsT=cqn[:, kc, m * 128:(m + 1) * 128], rhs=wuv[:, kc * 512:(kc + 1) * 512], start=(kc == 0), stop=(kc == 1)), r=[bcqn, bwuv], w=[bpt])
                ev, bev = ev_ring.next()
                A(lambda e: e.activation(out=ev[:], in_=pt[:], func=AF.Copy), r=[bpt], w=[bev])
                STO(xv[:, m, V_B:V_B + 512], ev[:], r=[bev], w=[bXL])

        def phase1_tm(l, tt):
            slot = tt
            xv = vview(XL, slot * RX + X_V)
            for m in range(4):
                def tmproj(c0, n):
                    pt, bpt = psr.next()
                    for kc in range(8):
                        P(lambda e: e.matmul(pt[:, 0:n], lhsT=hT[:, kc, m * 128:(m + 1) * 128], rhs=wtm[:, kc, c0:c0 + n], start=(kc == 0), stop=(kc == 7)), r=[bhT, bwtm], w=[bpt])
                    return pt, bpt
                pt, bpt = tmproj(0, 512)
                ev, bev = ev_ring.next()
                A(lambda e: e.activation(out=ev[:], in_=pt[:], func=AF.Copy, scale=rstd[:, m:m + 1]), r=[bpt, brstd], w=[bev])
                STO(xv[:, m, V_C:V_C + 512], ev[:], r=[bev], w=[bXL])
                pt, bpt = tmproj(512, 512)
                A(lambda e: e.activation(out=dvf[:], in_=pt[:], func=AF.Copy, scale=rstd[:, m:m + 1]), r=[bpt, brstd], w=[bdvf])
                V(lambda e: e.bn_stats(out=small[:, 8:14], in_=dvf[:]), r=[bdvf], w=[bsmall])
                V(lambda e: e.bn_aggr(out=small[:, 14:16], in_=small[:, 8:14]), r=[bsmall], w=[bsmall])
                V(lambda e: e.tensor_scalar(out=small[:, 16:17], in0=small[:, 15:16], scalar1=1e-5, scalar2=None, op0=ALU.add), r=[bsmall], w=[bsmall])
                RSQ(small[:, 16:17], small[:, 16:17], [bsmall], [bsmall])
                V(lambda e: e.tensor_scalar(out=dvf[:], in0=dvf[:], scalar1=small[:, 14:15], scalar2=small[:, 16:17], op0=ALU.subtract, op1=ALU.mult), r=[bdvf, bsmall], w=[bdvf])
                G(lambda e: e.tensor_tensor(out=dvf[:], in0=dvf[:], in1=lnp[:, 0, :], op=ALU.mult), r=[bdvf, blnp], w=[bdvf])
                G(lambda e: e.tensor_tensor(out=vcb[:], in0=dvf[:], in1=lnp[:, 1, :], op=ALU.add), r=[bdvf, blnp], w=[bvcb])
                pz, bpz = psr.next()
                for g in range(8):
                    P(lambda e: e.matmul(pz[(g % 2) * 64:(g % 2) * 64 + 64, (g // 2) * 128:(g // 2) * 128 + 128], lhsT=vcb[:, g * 64:(g + 1) * 64], rhs=wcT[:, g, :], start=True, stop=True), r=[bvcb, bwcT], w=[bpz])
                t1, bt1 = t1_ring.next()
                V(lambda e: e.tensor_tensor(out=t1[:], in0=pz[:], in1=bsT[:].rearrange("p a f -> p (a f)"), op=ALU.add), r=[bpz, bbsT], w=[bt1])
                G(lambda e: e.tensor_tensor(out=sgu[:, :, m * 128:(m + 1) * 128], in0=t1[:].rearrange("p (a f) -> p a f", a=4), in1=dUs[:, :, m * 128:(m + 1) * 128], op=ALU.mult), r=[bt1, bdU], w=[bsgu])
                pt, bpt = tmproj(1024, 68)
                ev, bev = ev_ring.next()
                A(lambda e: e.activation(out=ev[:, 0:64], in_=pt[:, 0:64], func=AF.Copy, scale=rstd[:, m:m + 1]), r=[bpt, brstd], w=[bev])
                STO(xv[:, m, V_A:V_A + 64], ev[:, 0:64], r=[bev], w=[bXL])
                V(lambda e: e.tensor_scalar(out=small[:, 20:24], in0=pt[:, 64:68], scalar1=rstd[:, m:m + 1], scalar2=1.0 / 16.0, op0=ALU.mult, op1=ALU.mult), r=[bpt, brstd], w=[bsmall])
                STO(iwtm[tt * 512 + m * 128: tt * 512 + (m + 1) * 128, :], small[:, 20:24], r=[bsmall], w=[biw])
                p2, bp2 = psr.next()
                P(lambda e: e.transpose(out=p2[0:4, 0:128], in_=small[:, 20:24], identity=ident_f[:]), r=[bsmall, bconst], w=[bp2])
                V(lambda e: e.tensor_copy(out=small[0:4, 24:24 + 0] if False else dvf[0:4, 0:128], in_=p2[0:4, 0:128]), r=[bp2], w=[bdvf])
                STO(iwT[:, tt * 512 + m * 128: tt * 512 + (m + 1) * 128], dvf[0:4, 0:128], r=[bdvf], w=[biw])
            for tq in range(4):
                STO(QS[Q_S + 128 * tq: Q_S + 128 * tq + 128, tt * 512:(tt + 1) * 512], sgu[:, tq, :], r=[bsgu], w=[bQS])

        sps = Ring(PS[3:6]); sps.b = bPS[3:6]
        OACC, bOACC = PS[6], bPS[6]
        LACC, bLACC = PS[7], bPS[7]
        dbgOB = dbg.get("OB")

        def mem_kv():
            LD(wbig[:], wkvb[:, :], r=[bwbf], w=[bwbig])
            for b in range(2):
                for hd in range(4):
                    pt, bpt = psr.next()
                    for kc in range(8):
                        P(lambda e: e.matmul(pt[:, 0:256], lhsT=wbig[:, kc * 1024 + hd * 128: kc * 1024 + hd * 128 + 128], rhs=memT[:, b, kc, :], start=(kc == 0), stop=(kc == 7)), r=[bwbig, bmemT], w=[bpt])
                    V(lambda e: e.tensor_copy(out=kmem[:, b, hd, :], in_=pt[:, 0:256]), r=[bpt], w=[bkvm])
                for mt in range(2):
                    pt, bpt = psr.next()
                    for kc in range(8):
                        P(lambda e: e.matmul(pt[:], lhsT=memT[:, b, kc, mt * 128:(mt + 1) * 128], rhs=wbig[:, kc * 1024 + 512: kc * 1024 + 1024], start=(kc == 0), stop=(kc == 7)), r=[bwbig, bmemT], w=[bpt])
                    V(lambda e: e.tensor_copy(out=vmem[:, b, mt, :], in_=pt[:]), r=[bpt], w=[bkvm])

        def load_q(row0, ntile, tt):
            for t in range(ntile):
                LD(qT[:, t, :], QS[row0 + 128 * t: row0 + 128 * t + 128, tt * 512:(tt + 1) * 512], r=[bQS], w=[bqT[t]])

        def attn_stream(b, i, q_ap, q_bufs, kparts, vspec, scale, mask_from_mbuf=False, want_L=False):
            nsb = 8 * i + 8
            loaded = {}

            def ensure(s):
                if s in loaded or s >= nsb:
                    return
                r_, il = s % 8, s // 8
                slot_k = b * NL + il
                kt, bkt = kT_ring.next()
                for (xr, n, p0) in kparts:
                    LD(kt[p0:p0 + n, :], xg_rows(r_, slot_k, xr, n), r=[bXG], w=[bkt])
                vt, bvt = vspec[2].next()
                xv = vview(XG, (r_ * NSLOT + slot_k) * RX + X_V)
                LD(vt[:, :, vspec[3]:vspec[3] + vspec[1]], xv[:, :, vspec[0]:vspec[0] + vspec[1]], r=[bXG], w=[bvt])
                mbt, bmbt = None, None
                if mask_from_mbuf:
                    mbt, bmbt = mb_ring.next()
                    LD(mbt[:], mbuf[:, s * 2048:(s + 1) * 2048].rearrange("p (t f) -> p t f", t=4), r=[bmbuf], w=[bmbt])
                loaded[s] = (kt, bkt, vt, bvt, mbt, bmbt)

            class _T:
                def __getitem__(self, idx):
                    s_, j_ = idx // 4, idx % 4
                    ensure(s_)
                    if j_ == 0:
                        ensure(s_ + 1)
                    return (s_, j_) + loaded[s_]
            tiles = _T()
            ntile = 4 * nsb
            k0, k1 = kparts[0][2], max(p0 + n for (_, n, p0) in kparts)
            stA = {}

            def stageA(idx):
                s, j, kt, bkt, vt, bvt, mbt, bmbt = tiles[idx]
                sp, bsp = sps.next()
                tail = s - 8 * i
                extra = []
                if mbt is not None:
                    extra.append((ident_b[:], mbt[:, j, :], [bmbt, bconst]))
                u0 = 31 - (4 * tail + j)
                extra.append((ident_b[:], stripT[:, u0 * 128:(u0 + 4) * 128], [bconst])) if tail >= 0 else None
                P(lambda e: e.matmul(sp[:], lhsT=kt[k0:k1, j * 128:(j + 1) * 128], rhs=q_ap, start=True, stop=(len(extra) == 0)), r=[bkt] + q_bufs, w=[bsp])
                for xi, (lt, rh, rb) in enumerate(extra):
                    P(lambda e: e.matmul(sp[:], lhsT=lt, rhs=rh, start=False, stop=(xi == len(extra) - 1)), r=rb, w=[bsp])
                stA[idx] = (sp, bsp)

            def stageBC(idx):
                s, j, kt, bkt, vt, bvt, mbt, bmbt = tiles[idx]
                sp, bsp = stA.pop(idx)
                pt, bpt = pT_ring.next()
                A(lambda e: e.activation(out=pt[:], in_=sp[:], func=AF.Exp, scale=scale), r=[bsp], w=[bpt])
                return pt, bpt

            pend = {}
            for idx in range(min(2, ntile)):
                stageA(idx)
            for idx in range(ntile):
                pt, bpt = stageBC(idx)
                if idx + 2 < ntile:
                    stageA(idx + 2)
                s, j, kt, bkt, vt, bvt, mbt, bmbt = tiles[idx]
                P(lambda e: e.matmul(OACC[:], lhsT=vt[:, j, :], rhs=pt[:], start=(idx == 0), stop=(idx == ntile - 1)), r=[bvt, bpt], w=[bOACC])
                if want_L:
                    P(lambda e: e.matmul(LACC[:], lhsT=ones_b[:], rhs=pt[:], start=(idx == 0), stop=(idx == ntile - 1)), r=[bpt, bconst], w=[bLACC])

        bmbuf = Buf()

        def norm_half(dst_ap, dst_buf, hh):
            n0, l0 = 64 * hh, 64 * (1 - hh)
            V(lambda e: e.reciprocal(rrec[l0:l0 + 64, :], OACC[l0:l0 + 64, :]), r=[bOACC], w=[brrec])
            V(lambda e: e.tensor_tensor(out=dst_ap, in0=OACC[n0:n0 + 64, :], in1=rrec[l0:l0 + 64, :], op=ALU.mult), r=[bOACC, brrec], w=[dst_buf])

        def finish_branch(n, tt, first):
            if dbgOB is not None:
                for tq in range(4):
                    STO(dbgOB[n, tq * 128:(tq + 1) * 128, tt * 512:(tt + 1) * 512], obr[:, tq, :], r=[bobr], w=[Buf()])
            for tq in range(4):
                gt, bgt = gt_ring.next()
                LD(gt[:], QS[Q_G + n * 512 + tq * 128: Q_G + n * 512 + tq * 128 + 128, tt * 512:(tt + 1) * 512], r=[bQS], w=[bgt])
                t1, bt1 = t1_ring.next()
                A(lambda e: e.activation(out=t1[:], in_=gt[:], func=AF.Silu), r=[bgt], w=[bt1])
                V(lambda e: e.tensor_tensor(out=ogT[:, tq, :], in0=obr[:, tq, :], in1=t1[:], op=ALU.mult), r=[bobr, bt1], w=[bogT])
            LD(wbig[:, 0:4096], wbrb[n], r=[bwbf], w=[bwbig])
            for ct in range(8):
                pt, bpt = psr.next()
                for kc in range(4):
                    P(lambda e: e.matmul(pt[:], lhsT=wbig[:, kc * 1024 + ct * 128: kc * 1024 + ct * 128 + 128], rhs=ogT[:, kc, :], start=(kc == 0), stop=(kc == 3)), r=[bwbig, bogT], w=[bpt])
                gt, bgt = gt_ring.next()
                LD(gt[:], QS[Q_M + n * 1024 + ct * 128: Q_M + n * 1024 + ct * 128 + 128, tt * 512:(tt + 1) * 512], r=[bQS], w=[bgt])
                t1, bt1 = t1_ring.next()
                A(lambda e: e.activation(out=t1[:], in_=gt[:], func=AF.Sigmoid), r=[bgt], w=[bt1])
                if first:
                    V(lambda e: e.tensor_tensor(out=mixT[:, ct, :], in0=pt[:], in1=t1[:], op=ALU.mult), r=[bpt, bt1], w=[bmix])
                else:
                    V(lambda e: e.tensor_tensor(out=t1[:], in0=pt[:], in1=t1[:], op=ALU.mult), r=[bpt, bt1], w=[bt1])
                    G(lambda e: e.tensor_tensor(out=mixT[:, ct, :], in0=mixT[:, ct, :], in1=t1[:], op=ALU.add), r=[bt1, bmix], w=[bmix])

        def phase2_idx(l, b, i):
            tt = b * NL + i
            nsb = 8 * i + 8
            nkt = 4 * nsb
            load_q(Q_I, 2, tt)
            LD(iwq[:], iwtm[tt * 512:(tt + 1) * 512, :].rearrange("(m p) h -> p m h", p=128), r=[biw], w=[biwq])
            LD(iwr[:], iwT[:, tt * 512:(tt + 1) * 512], r=[biw], w=[biwr])
            for m in range(4):
                for s in range(nsb):
                    r_, il = s % 8, s // 8
                    kt, bkt = kT_ring.next()
                    LD(kt[:], xg_rows(r_, b * NL + il, X_KI, 128), r=[bXG], w=[bkt])
                    for hI in range(4):
                        pt, bpt = psr.next()
                        p0 = 64 * (hI % 2)
                        P(lambda e: e.matmul(pt[:], lhsT=qT[p0:p0 + 64, hI // 2, m * 128:(m + 1) * 128], rhs=kt[p0:p0 + 64, :], start=True, stop=True), r=[bkt, bqT[hI // 2]], w=[bpt])
                        rv = rows[:, s * 512:(s + 1) * 512]
                        if hI == 0:
                            V(lambda e: e.tensor_scalar(out=rv, in0=pt[:], scalar1=0.0, scalar2=iwq[:, m, 0:1], op0=ALU.max, op1=ALU.mult), r=[bpt, biwq], w=[brows])
                        else:
                            V(lambda e: e.tensor_scalar(out=tmpT[:], in0=pt[:], scalar1=0.0, scalar2=iwq[:, m, hI:hI + 1], op0=ALU.max, op1=ALU.mult), r=[bpt, biwq], w=[btmpT])
                            G(lambda e: e.tensor_tensor(out=rv, in0=rv, in1=tmpT[:], op=ALU.add), r=[btmpT, brows], w=[brows])
                tv = rows[:, (nkt - 32) * 128: nkt * 128]
                V(lambda e: e.tensor_tensor(out=tv, in0=tv, in1=stripQ[:, (3 - m) * 128:(3 - m + 32) * 128], op=ALU.add), r=[brows, bconst], w=[brows])
                for rnd in range(32):
                    V(lambda e: e.max(out=m8[:], in_=rows[:, 0:nkt * 128]), r=[brows], w=[bm8])
                    if rnd < 31:
                        V(lambda e: e.match_replace(out=rows[:, 0:nkt * 128], in_to_replace=m8[:], in_values=rows[:, 0:nkt * 128], imm_value=-3.0e38), r=[bm8, brows], w=[brows])
                if "TH" in dbg:
                    STO(dbg["TH"][tt * 512 + m * 128: tt * 512 + (m + 1) * 128, :], m8[:, 7:8], r=[bm8], w=[Buf()])
                V(lambda e: e.tensor_scalar(out=dvf[:, 0:128], in0=ident_f[:], scalar1=m8[:, 7:8], scalar2=None, op0=ALU.mult), r=[bconst, bm8], w=[bdvf])
                pt, bpt = psr.next()
                P(lambda e: e.matmul(pt[:, 0:128], lhsT=ones_f[:], rhs=dvf[:, 0:128], start=True, stop=True), r=[bdvf, bconst], w=[bpt])
                V(lambda e: e.tensor_copy(out=tbc[:, m * 128:(m + 1) * 128], in_=pt[:, 0:128]), r=[bpt], w=[btbc])
            for hI in range(4):
                pt, bpt = psr.next()
                P(lambda e: e.matmul(pt[:], lhsT=selT[:, hI, :], rhs=iwr[:], start=True, stop=True), r=[biwr, bconst], w=[bpt])
                V(lambda e: e.tensor_copy(out=wbc[:, hI, :], in_=pt[:]), r=[bpt], w=[bwbc])
            for s in range(nsb):
                r_, il = s % 8, s // 8
                kt, bkt = kT_ring.next()
                LD(kt[:], xg_rows(r_, b * NL + il, X_KI, 128), r=[bXG], w=[bkt])
                mbt, bmbt = mb_ring.next()
                for j in range(4):
                    for hI in range(4):
                        pt, bpt = psr.next()
                        p0 = 64 * (hI % 2)
                        P(lambda e: e.matmul(pt[:], lhsT=kt[p0:p0 + 64, j * 128:(j + 1) * 128], rhs=qT[p0:p0 + 64, hI // 2, :], start=True, stop=True), r=[bkt, bqT[hI // 2]], w=[bpt])
                        if hI == 0:
                            V(lambda e: e.scalar_tensor_tensor(out=accT[:], in0=pt[:], scalar=0.0, in1=wbc[:, 0, :], op0=ALU.max, op1=ALU.mult), r=[bpt, bwbc], w=[baccT])
                        else:
                            V(lambda e: e.scalar_tensor_tensor(out=tmpT[:], in0=pt[:], scalar=0.0, in1=wbc[:, hI, :], op0=ALU.max, op1=ALU.mult), r=[bpt, bwbc], w=[btmpT])
                            G(lambda e: e.tensor_tensor(out=accT[:], in0=accT[:], in1=tmpT[:], op=ALU.add), r=[btmpT, baccT], w=[baccT])
                    V(lambda e: e.tensor_tensor(out=accT[:], in0=accT[:], in1=tbc[:], op=ALU.subtract), r=[baccT, btbc], w=[baccT])
                    V(lambda e: e.tensor_scalar(out=mbt[:, j, :], in0=accT[:], scalar1=1.0e30, scalar2=0.0, op0=ALU.mult, op1=ALU.min), r=[baccT], w=[bmbt])
                STO(mbuf[:, s * 2048:(s + 1) * 2048].rearrange("p (t f) -> p t f", t=4), mbt[:], r=[bmbt], w=[bmbuf])
        def phase2_rest(l, b, i, lam_init, last):
            tt = b * NL + i
            nsb = 8 * i + 8
            nkt = 4 * nsb
            load_q(Q_A, 4, tt)
            for hA in range(8):
                hh = hA % 2
                ring = VA_ring if hh == 0 else VB_ring
                attn_stream(b, i, qT[64 * hh:64 * hh + 64, hA // 2, :], [bqT[hA // 2]], [(X_KA + 64 * hh, 64, 64 * hh)],
                            (V_A, 64, ring, 64 * hh), 0.125, mask_from_mbuf=True)
                norm_half(obr[64 * hh:64 * hh + 64, hA // 2, :], bobr, hh)
            finish_branch(0, tt, True)
            load_q(Q_B, 8, tt)
            for hB in range(8):
                hh = hB % 2
                ring = VA_ring if hh == 0 else VB_ring
                attn_stream(b, i, qT[0:96, hB, :], [bqT[hB]], [(X_KB + 64 * hB, 64, 0), (X_KR, 32, 64)],
                            (V_B + 64 * hB, 64, ring, 64 * hh), 96.0 ** -0.5)
                norm_half(obr[64 * hh:64 * hh + 64, hB // 2, :], bobr, hh)
            finish_branch(1, tt, False)
            load_q(Q_C, 4, tt)
            for hd in range(4):
                for sg in range(2):
                    cc_ = 2 * hd + sg
                    attn_stream(b, i, qT[64 * sg:64 * sg + 64, hd, :], [bqT[hd]], [(X_KC + 64 * cc_, 64, 64 * sg)],
                                (V_C + 128 * hd, 128, VC_ring, 0), 0.125, want_L=True)
                    V(lambda e: e.reciprocal(rrec[:], LACC[:]), r=[bLACC], w=[brrec])
                    V(lambda e: e.tensor_tensor(out=dtmp[:, sg, :], in0=OACC[:], in1=rrec[:], op=ALU.mult), r=[bOACC, brrec], w=[bdtmp])
                V(lambda e: e.scalar_tensor_tensor(out=dtmp[:, 0, :], in0=dtmp[:, 1, :], scalar=lam[:, 1:2], in1=dtmp[:, 0, :], op0=ALU.mult, op1=ALU.add), r=[bdtmp, blam], w=[bdtmp])
                V(lambda e: e.tensor_tensor(out=dtmp[:, 1, :], in0=dtmp[:, 0, :], in1=dtmp[:, 0, :], op=ALU.mult), r=[bdtmp], w=[bdtmp])
                pt, bpt = psr.next()
                P(lambda e: e.matmul(pt[:], lhsT=ones_f[:], rhs=dtmp[:, 1, :], start=True, stop=True), r=[bdtmp, bconst], w=[bpt])
                V(lambda e: e.tensor_scalar(out=dtmp[:, 1, :], in0=pt[:], scalar1=1.0 / 128.0, scalar2=1e-6, op0=ALU.mult, op1=ALU.add), r=[bpt], w=[bdtmp])
                RSQ(dtmp[:, 1, :], dtmp[:, 1, :], [bdtmp], [bdtmp])
                V(lambda e: e.tensor_scalar(out=dtmp[:, 1, :], in0=dtmp[:, 1, :], scalar1=(1.0 - lam_init), scalar2=None, op0=ALU.mult), r=[bdtmp], w=[bdtmp])
                V(lambda e: e.scalar_tensor_tensor(out=obr[:, hd, :], in0=dtmp[:, 0, :], scalar=dng[:, 0:1], in1=dtmp[:, 1, :], op0=ALU.mult, op1=ALU.mult), r=[bdtmp, blw], w=[bobr])
            finish_branch(2, tt, False)
            for tq in range(4):
                gt, bgt = gt_ring.next()
                LD(gt[:], QS[Q_S + 128 * tq: Q_S + 128 * tq + 128, tt * 512:(tt + 1) * 512], r=[bQS], w=[bgt])
                V(lambda e: e.tensor_copy(out=obr[:, tq, :], in_=gt[:]), r=[bgt], w=[bobr])
            finish_branch(3, tt, False)
            load_q(Q_E, 4, tt)
            for hE in range(4):
                pts = []
                for mt in range(2):
                    sp, bsp = sps.next()
                    P(lambda e: e.matmul(sp[:], lhsT=kmem[:, b, hE, mt * 128:(mt + 1) * 128], rhs=qT[:, hE, :], start=True, stop=True), r=[bkvm, bqT[hE]], w=[bsp])
                    pt, bpt = pT_ring.next()
                    A(lambda e: e.activation(out=pt[:], in_=sp[:], func=AF.Exp, scale=128.0 ** -0.5), r=[bsp], w=[bpt])
                    pts.append((pt, bpt))
                for mt, (pt, bpt) in enumerate(pts):
                    P(lambda e: e.matmul(OACC[:], lhsT=vmem[:, b, mt, hE * 128:(hE + 1) * 128], rhs=pt[:], start=(mt == 0), stop=(mt == 1)), r=[bkvm, bpt], w=[bOACC])
                    P(lambda e: e.matmul(LACC[:], lhsT=ones_b[:], rhs=pt[:], start=(mt == 0), stop=(mt == 1)), r=[bpt, bconst], w=[bLACC])
                V(lambda e: e.reciprocal(rrec[:], LACC[:]), r=[bLACC], w=[brrec])
                V(lambda e: e.tensor_tensor(out=obr[:, hE, :], in0=OACC[:], in1=rrec[:], op=ALU.mult), r=[bOACC, brrec], w=[bobr])
            finish_branch(4, tt, False)
            for kc in range(8):
                V(lambda e: e.tensor_copy(out=mixb[:, kc, :], in_=mixT[:, kc, :]), r=[bmix], w=[bmixb])
            LD(wbig[:], woutb[:, :], r=[bwbf], w=[bwbig])
            for m in range(4):
                hv = stage[:, 0:1024]
                LD(hv, hbuf[tt * 512 + m * 128: tt * 512 + (m + 1) * 128, :], r=[bh], w=[bstage])
                for ch in range(2):
                    pt, bpt = psr.next()
                    for kc in range(8):
                        P(lambda e: e.matmul(pt[:], lhsT=mixb[:, kc, m * 128:(m + 1) * 128], rhs=wbig[:, kc * 1024 + ch * 512: kc * 1024 + ch * 512 + 512], start=(kc == 0), stop=(kc == 7)), r=[bmixb, bwbig], w=[bpt])
                    V(lambda e: e.tensor_tensor(out=hv[:, ch * 512:(ch + 1) * 512], in0=pt[:], in1=hv[:, ch * 512:(ch + 1) * 512], op=ALU.add), r=[bpt, bstage], w=[bstage])
                if not last:
                    STO(hbuf[tt * 512 + m * 128: tt * 512 + (m + 1) * 128, :], hv, r=[bstage], w=[bh])
                else:
                    A(lambda e: e.activation(out=junk[:], in_=hv, func=AF.Square, accum_out=small[:, 30:31]), r=[bstage], w=[bjunk, bsmall])
                    V(lambda e: e.tensor_scalar(out=small[:, 31:32], in0=small[:, 30:31], scalar1=1.0 / D, scalar2=1e-6, op0=ALU.mult, op1=ALU.add), r=[bsmall], w=[bsmall])
                    RSQ(small[:, 31:32], small[:, 31:32], [bsmall], [bsmall])
                    V(lambda e: e.scalar_tensor_tensor(out=hv, in0=hv, scalar=small[:, 31:32], in1=gbc[:], op0=ALU.mult, op1=ALU.mult), r=[bstage, bsmall, bgbc], w=[bstage])
                    STO(y_out[tt * 512 + m * 128: tt * 512 + (m + 1) * 128, :], hv, r=[bstage], w=[byout])

        byout = Buf()
        for hI in range(4):
            V(lambda e: e.tensor_scalar(out=selT[:, hI, :], in0=ones_f[0:4, :], scalar1=ident_f[0:4, hI:hI + 1], scalar2=None, op0=ALU.mult), r=[bconst], w=[bconst])

        for l in range(depth):
            last = (l == depth - 1)
            s1 = ExitStack()
            cur[0] = s1
            hT = sb("hT", [128, 8, 512], BF16)
            bhT = Buf()
            rstd = sb("rstd", [128, 4], F32)
            brstd = Buf()
            rbc = sb("rbc", [128, 512], F32)
            brbc = Buf()
            tabs = sb("tabs", [128, 4, 512], F32)
            btabs = Buf()
            wt_ring = Ring([sb("wt%d" % i, [128, 8 * 128], BF16) for i in range(4)])
            ev_ring = Ring([sb("ev%d" % i, [128, 512], BF16) for i in range(4)])
            cqf = sb("cqf", [128, 3, 512], F32)
            bcqf = Buf()
            cqn = sb("cqn", [128, 3, 512], BF16)
            bcqn = Buf()
            dUs = sb("dUs", [128, 4, 512], BF16)
            bdU = Buf()
            sgu = sb("sgu", [128, 4, 512], BF16)
            bsgu = Buf()
            vcb = sb("vcb", [128, 512], BF16)
            bvcb = Buf()
            wsm = sb("wsm", [128, 3 * 128], BF16)
            wsm_ring = Ring([wsm, sb("wsm2", [128, 3 * 128], BF16)])
            wuv = sb("wuv", [128, 2 * 512], BF16)
            bwuv = Buf()
            wtm = sb("wtm", [128, 8, 1152], BF16)
            bwtm = Buf()
            lnp = sb("lnp", [128, 2, 512], F32)
            blnp = Buf()
            wcT = sb("wcT", [128, 8, 128], BF16)
            bwcT = Buf()
            bsT = sb("bsT", [128, 4, 128], F32)
            bbsT = Buf()
            lam_init = prep_layer(l)
            if last:
                LD(gbc[:], fng_in.partition_broadcast(128), w=[bgbc])
            for tt in range(NSLOT):
                phase1(l, tt)
                phase1_tm(l, tt)
            c.barrier(["gpsimd"])
            cc_count[0] += 1
            nc.gpsimd.collective_compute("AllGather", ALU.bypass, replica_groups=[list(range(NCORE))],
                                         ins=[XL], outs=[XG]).then_inc(ccs, 1)
            nc.gpsimd.wait_ge(ccs, cc_count[0])
            cctok = G(lambda e: e.memset(small[:, 60:61], 0.0), w=[bsmall])
            bXG.w = cctok
            bXG.r = []
            bXL.r.append(cctok)
            c.barrier()
            s1.close()
            s2 = ExitStack()
            cur[0] = s2
            qT = sb("qT", [128, 8, 512], BF16)
            bqT = [Buf() for _ in range(8)]
            kT_ring = Ring([sb("kT%d" % i, [128, 512], BF16) for i in range(3)])
            VA_ring = Ring([sb("VA%d" % i, [128, 4, 128], BF16) for i in range(3)])
            VB_ring = Ring([sb("VB%d" % i, [128, 4, 128], BF16) for i in range(3)])
            for t in VA_ring.t:
                G(lambda e: e.memset(t[:, :, 64:128], 1.0), w=[VA_ring.b[VA_ring.t.index(t)]])
            for t in VB_ring.t:
                G(lambda e: e.memset(t[:, :, 0:64], 1.0), w=[VB_ring.b[VB_ring.t.index(t)]])
            pT_ring = Ring([sb("pT%d" % i, [128, 512], BF16) for i in range(3)])
            mb_ring = Ring([sb("mb%d" % i, [128, 4, 512], BF16) for i in range(2)])
            kmem = sb("kmem", [128, 2, 4, 256], BF16)
            vmem = sb("vmem", [128, 2, 2, 512], BF16)
            bkvm = Buf()
            wbig = sb("wbig", [128, 8 * 1024], BF16)
            bwbig = Buf()
            VC_ring = Ring([sb("VC%d" % i, [128, 4, 128], BF16) for i in range(3)])
            mem_kv()
            for b in range(2):
                for i in range(NL):
                    s3 = ExitStack()
                    cur[0] = s3
                    rows = sb("rows", [128, NKMAX * 128], F32)
                    brows = Buf()
                    m8 = sb("m8", [128, 8], F32)
                    bm8 = Buf()
                    tbc = sb("tbc", [128, 512], F32)
                    btbc = Buf()
                    wbc = sb("wbc", [128, 4, 512], F32)
                    bwbc = Buf()
                    iwq = sb("iwq", [128, 4, 4], F32)
                    biwq = Buf()
                    iwr = sb("iwr", [4, 512], F32)
                    biwr = Buf()
                    accT = sb("accT", [128, 512], F32)
                    baccT = Buf()
                    tmpT = sb("tmpT", [128, 512], F32)
                    btmpT = Buf()
                    phase2_idx(l, b, i)
                    c.barrier()
                    s3.close()
                    s4 = ExitStack()
                    cur[0] = s4
                    obr = sb("obr", [128, 4, 512], F32)
                    bobr = Buf()
                    ogT = sb("ogT", [128, 4, 512], BF16)
                    bogT = Buf()
                    mixT = sb("mixT", [128, 8, 512], F32)
                    bmix = Buf()
                    mixb = sb("mixb", [128, 8, 512], BF16)
                    bmixb = Buf()
                    gt_ring = Ring([sb("gt%d" % i, [128, 512], BF16) for i in range(2)])
                    rrec = sb("rrec", [128, 512], F32)
                    brrec = Buf()
                    dtmp = sb("dtmp", [128, 2, 512], F32)
                    bdtmp = Buf()
                    phase2_rest(l, b, i, lam_init, last)
                    c.barrier()
                    s4.close()
            if debug and l == 0:
                for r0 in range(0, NQ, 128):
                    n = min(128, NQ - r0)
                    for c0 in range(0, T, 2048):
                        w_ = min(2048, T - c0)
                        tv = junk[0:n, 0:w_ // 2] if False else None
                        for c1 in range(c0, c0 + w_, 1024):
                            LD(junk[0:n, 0:1024], QS[r0:r0 + n, c1:c1 + 1024], r=[bQS], w=[bjunk])
                            STO(dbg["QS"][r0:r0 + n, c1:c1 + 1024], junk[0:n, 0:1024], r=[bjunk], w=[Buf()])
                for r0 in range(0, NSLOT * RX, 128):
                    n = min(128, NSLOT * RX - r0)
                    LD(junk[0:n, 0:512], XL[r0:r0 + n, :], r=[bXL], w=[bjunk])
                    STO(dbg["XL"][r0:r0 + n, :], junk[0:n, 0:512], r=[bjunk], w=[Buf()])
            c.barrier()
            s2.close()

        c.barrier(["sync", "gpsimd"])
    return nc


def _prep_inputs(S, depth, x, mem, positions, norm_g, w_in, mla_q_norm_g, mla_kv_norm_g, mla_w_uq, mla_w_ukv,
                 diff_lambda, diff_norm_g, sgu_ln_g, sgu_ln_b, sgu_w, sgu_b, mem_norm_g, mem_w_kv,
                 w_branch, w_out, final_norm_g):
    f32 = np.float32
    NSB = S // 512
    NL = NSB // NCORE
    L = depth
    widx = _win_index()
    w_in_p = np.concatenate([np.asarray(w_in[:L], f32), np.zeros((L, D, 1), f32)], axis=2)[:, :, widx]
    w_uq_p = np.concatenate([np.asarray(mla_w_uq[:L], f32), np.zeros((L, 384, 1), f32)], axis=2)[:, :, _uq_index()]
    w_ukv_p = np.asarray(mla_w_ukv[:L], f32)[:, :, _ukv_index()]
    common = {
        "mem": np.ascontiguousarray(mem, f32),
        "ropec": _rope_consts(),
        "norm_g": np.ascontiguousarray(np.asarray(norm_g[:L], f32).reshape(L, 8, 128).transpose(0, 2, 1)),
        "w_in": np.ascontiguousarray(w_in_p),
        "qg": np.ascontiguousarray(np.asarray(mla_q_norm_g[:L], f32).reshape(L, 3, 128).transpose(0, 2, 1)),
        "kvg": np.ascontiguousarray(np.asarray(mla_kv_norm_g[:L], f32).reshape(L, 2, 128).transpose(0, 2, 1)),
        "w_uq": np.ascontiguousarray(w_uq_p),
        "w_ukv": np.ascontiguousarray(w_ukv_p),
        "dlam": np.ascontiguousarray(diff_lambda[:L], f32),
        "dng": np.ascontiguousarray(np.asarray(diff_norm_g[:L], f32).reshape(L, 128, 1)),
        "sgu_g": np.ascontiguousarray(sgu_ln_g[:L], f32),
        "sgu_b": np.ascontiguousarray(sgu_ln_b[:L], f32),
        "sgu_wT": np.ascontiguousarray(np.asarray(sgu_w[:L], f32).transpose(0, 1, 3, 2)),
        "sgu_bs": np.ascontiguousarray(sgu_b[:L], f32),
        "mem_g": np.ascontiguousarray(mem_norm_g, f32),
        "w_kv": np.ascontiguousarray(mem_w_kv[:L], f32),
        "w_br": np.ascontiguousarray(w_branch[:L], f32),
        "w_out": np.ascontiguousarray(w_out[:L], f32),
        "fin_g": np.ascontiguousarray(final_norm_g, f32),
    }
    x = np.asarray(x, f32)
    positions = np.asarray(positions, np.int32)
    in_maps = []
    for c in range(NCORE):
        xs, ps = [], []
        for b in range(2):
            for i in range(NL):
                s = 8 * i + c
                xs.append(x[b, s * 512:(s + 1) * 512])
                ps.append(positions[b, s * 512:(s + 1) * 512])
        m = dict(common)
        m["x"] = np.ascontiguousarray(np.concatenate(xs, 0))
        m["pos"] = np.ascontiguousarray(np.concatenate(ps, 0))
        m["cidx"] = np.full((128, 1), 512.0 * c, f32)
        in_maps.append(m)
    return in_maps


def _gather_out(S, res, key="y"):
    NSB = S // 512
    NL = NSB // NCORE
    out = np.zeros((2, S, D), np.float32)
    for c in range(NCORE):
        y = np.asarray(res[c][key], np.float32)
        k = 0
        for b in range(2):
            for i in range(NL):
                s = 8 * i + c
                out[b, s * 512:(s + 1) * 512] = y[k * 512:(k + 1) * 512]
                k += 1
    return out


_CACHE = {}


def run(S, depth, inputs, debug=False):
    key = (S, depth, debug)
    if key not in _CACHE:
        _CACHE[key] = build_program(S, depth, debug)
    nc = _CACHE[key]
    in_maps = _prep_inputs(S, depth, **inputs)
    res = run_bass_kernel_spmd(nc, in_maps, core_ids=list(range(NCORE)))
    return res.results


def kernel(**inputs):
    S = inputs["x"].shape[1]
    res = run(S, 4, inputs)
    return _gather_out(S, res)
```
